# Optimizing a Trainium2 kernel written in Bass

```python
import math
import jax
import jax.numpy as jnp
from jax import lax
import numpy as np


D_MODEL = 1024
BATCH = 4
SEQ = 4096
DEPTH = 2

SSD_HEADS = 16
SSD_HEAD_DIM = 64
D_SSD = SSD_HEADS * SSD_HEAD_DIM
SSD_GROUPS = 2
SSD_STATE = 128
SSD_CONV = 4
SSD_CHUNK = 256
CONV_DIM = D_SSD + 2 * SSD_GROUPS * SSD_STATE
ATT_HEADS = 8
ATT_HEAD_DIM = 128
D_ATT = ATT_HEADS * ATT_HEAD_DIM
IDX_HEADS = 8
IDX_HEAD_DIM = 64
TOPK_MAX = 256
Q_BLOCK = 128
ROPE_THETA = 500000.0
ATT_ROPE_DIM = ATT_HEAD_DIM // 4
IDX_ROPE_DIM = IDX_HEAD_DIM // 4
D_MIX = D_SSD + D_ATT
D_FF = 2816
EPS = 1e-6
MIX_SPLITS = (D_SSD, CONV_DIM, SSD_HEADS, D_ATT, D_ATT, D_ATT,
              IDX_HEADS * IDX_HEAD_DIM, IDX_HEAD_DIM, IDX_HEADS)
SPLIT_POINTS = tuple(int(p) for p in np.cumsum(MIX_SPLITS)[:-1])
D_IN_PROJ = sum(MIX_SPLITS)

kernel_name = 'hybrid_ssd_dsa_macaron'


def rmsnorm(x, g):
    xf = x.astype(jnp.float32)
    y = xf * lax.rsqrt(jnp.mean(xf * xf, axis=-1, keepdims=True) + EPS)
    return (y * g.astype(jnp.float32)).astype(x.dtype)


def swiglu_ffn(x, w_in, w_out):
    gate, up = jnp.split(x @ w_in, 2, axis=-1)
    return (jax.nn.silu(gate) * up) @ w_out


def partial_rope(x, positions, rot_dim):
    inv_freq = ROPE_THETA ** (-jnp.arange(0, rot_dim, 2, dtype=jnp.float32) / rot_dim)
    ang = positions.astype(jnp.float32)[..., None] * inv_freq
    cos = jnp.cos(ang)[:, :, None, :]
    sin = jnp.sin(ang)[:, :, None, :]
    xr = x[..., :rot_dim].astype(jnp.float32)
    x1, x2 = jnp.split(xr, 2, axis=-1)
    rot = jnp.concatenate([x1 * cos - x2 * sin, x2 * cos + x1 * sin], axis=-1)
    return jnp.concatenate([rot.astype(x.dtype), x[..., rot_dim:]], axis=-1)


def causal_dwconv(x, w, b):
    k = w.shape[0]
    y = lax.conv_general_dilated(x, w[:, None, :].astype(x.dtype), window_strides=(1,),
                                 padding=[(k - 1, 0)],
                                 dimension_numbers=('NWC', 'WIO', 'NWC'),
                                 feature_group_count=x.shape[-1])
    return y + b.astype(x.dtype)


def segsum_exp(a):
    l = a.shape[-1]
    cs = jnp.cumsum(a, axis=-1)
    seg = cs[..., :, None] - cs[..., None, :]
    mask = jnp.tril(jnp.ones((l, l), dtype=bool))
    return jnp.exp(jnp.where(mask, seg, -jnp.inf))


def ssd_chunked(xh, dt, a, bmat, cmat, chunk):
    bsz, L, H, P = xh.shape
    G, N = bmat.shape[-2:]
    E = H // G
    nc = L // chunk
    x = (xh.astype(jnp.float32) * dt[..., None]).reshape(bsz, nc, chunk, G, E, P)
    adt = jnp.moveaxis((dt * a).reshape(bsz, nc, chunk, G, E), 2, -1)
    a_cs = jnp.cumsum(adt, axis=-1)
    bc = bmat.astype(jnp.float32).reshape(bsz, nc, chunk, G, N)
    cc = cmat.astype(jnp.float32).reshape(bsz, nc, chunk, G, N)
    decay = segsum_exp(adt)
    cb = jnp.einsum('bclgn,bcsgn->bcgls', cc, bc)
    y_diag = jnp.einsum('bcgls,bcgels,bcsgep->bclgep', cb, decay, x)
    decay_states = jnp.exp(a_cs[..., -1:] - a_cs)
    states = jnp.einsum('bclgn,bcgel,bclgep->bcgepn', bc, decay_states, x)
    chunk_decay = jnp.exp(a_cs[..., -1])

    def step(h, inp):
        st, dec = inp
        return h * dec[..., None, None] + st, h

    h0 = jnp.zeros((bsz, G, E, P, N), jnp.float32)
    _, h_in = lax.scan(step, h0, (jnp.moveaxis(states, 1, 0), jnp.moveaxis(chunk_decay, 1, 0)))
    h_in = jnp.moveaxis(h_in, 0, 1)
    y_off = jnp.einsum('bclgn,bcgepn,bcgel->bclgep', cc, h_in, jnp.exp(a_cs))
    return (y_diag + y_off).reshape(bsz, L, H, P)


def ssd_mixer(z, xbc, dt_raw, conv_w, conv_b, dt_bias, a_log, d_skip, norm_g):
    bsz, L, _ = z.shape
    xbc = jax.nn.silu(causal_dwconv(xbc, conv_w, conv_b))
    xs, bm, cm = jnp.split(xbc, [D_SSD, D_SSD + SSD_GROUPS * SSD_STATE], axis=-1)
    xh = xs.reshape(bsz, L, SSD_HEADS, SSD_HEAD_DIM)
    bm = bm.reshape(bsz, L, SSD_GROUPS, SSD_STATE)
    cm = cm.reshape(bsz, L, SSD_GROUPS, SSD_STATE)
    dt = jax.nn.softplus(dt_raw.astype(jnp.float32) + dt_bias.astype(jnp.float32))
    a = -jnp.exp(a_log.astype(jnp.float32))
    chunk = math.gcd(SSD_CHUNK, L)
    y = ssd_chunked(xh, dt, a, bm, cm, chunk)
    y = y + xh.astype(jnp.float32) * d_skip.astype(jnp.float32)[:, None]
    y = y.reshape(bsz, L, D_SSD) * jax.nn.silu(z.astype(jnp.float32))
    yg = y.reshape(bsz, L, SSD_GROUPS, D_SSD // SSD_GROUPS)
    yg = yg * lax.rsqrt(jnp.mean(yg * yg, axis=-1, keepdims=True) + EPS)
    return (yg.reshape(bsz, L, D_SSD) * norm_g.astype(jnp.float32)).astype(z.dtype)


def dsa_mixer(q, k, v, q_idx, k_idx, w_idx, positions):
    bsz, L, _ = q.shape
    q = partial_rope(q.reshape(bsz, L, ATT_HEADS, ATT_HEAD_DIM), positions, ATT_ROPE_DIM)
    k = partial_rope(k.reshape(bsz, L, ATT_HEADS, ATT_HEAD_DIM), positions, ATT_ROPE_DIM)
    v = v.reshape(bsz, L, ATT_HEADS, ATT_HEAD_DIM)
    qi = partial_rope(q_idx.reshape(bsz, L, IDX_HEADS, IDX_HEAD_DIM), positions, IDX_ROPE_DIM)
    ki = partial_rope(k_idx[:, :, None, :], positions, IDX_ROPE_DIM)[:, :, 0].astype(jnp.float32)
    wi = w_idx.astype(jnp.float32) * IDX_HEADS ** -0.5
    topk = min(TOPK_MAX, L // 4)
    n_blocks = L // Q_BLOCK
    key_pos = jnp.arange(L)
    att_scale = ATT_HEAD_DIM ** -0.5
    idx_scale = IDX_HEAD_DIM ** -0.5

    def block(i):
        start = i * Q_BLOCK
        qb = lax.dynamic_slice_in_dim(q, start, Q_BLOCK, axis=1)
        qib = lax.dynamic_slice_in_dim(qi, start, Q_BLOCK, axis=1)
        wb = lax.dynamic_slice_in_dim(wi, start, Q_BLOCK, axis=1)
        t = start + jnp.arange(Q_BLOCK)
        causal = key_pos[None, :] <= t[:, None]
        logit_idx = jnp.einsum('bthd,bsd->bths', qib.astype(jnp.float32), ki) * idx_scale
        score = jnp.einsum('bth,bths->bts', wb, jax.nn.relu(logit_idx))
        score = jnp.where(causal[None], score, -jnp.inf)
        _, sel = lax.top_k(score, topk)
        k_sel = jax.vmap(lambda kb, ib: kb[ib])(k, sel)
        v_sel = jax.vmap(lambda vb, ib: vb[ib])(v, sel)
        valid = sel <= t[None, :, None]
        s = jnp.einsum('bthd,btkhd->bhtk', qb.astype(jnp.float32),
                       k_sel.astype(jnp.float32)) * att_scale
        s = jnp.where(valid[:, None], s, -jnp.inf)
        p = jax.nn.softmax(s, axis=-1)
        o = jnp.einsum('bhtk,btkhd->bthd', p, v_sel.astype(jnp.float32))
        return o.astype(q.dtype)

    out = lax.map(block, jnp.arange(n_blocks))
    return jnp.moveaxis(out, 0, 1).reshape(bsz, L, D_ATT)


def setup_inputs(seed: int = 0) -> dict:
    key = jax.random.key(seed)
    ks = jax.random.split(key, 20)
    f32 = jnp.float32

    def dense(k, shape, fan_in):
        return jax.random.normal(k, shape, f32) * fan_in ** -0.5

    def gain(k, shape):
        return 1.0 + 0.02 * jax.random.normal(k, shape, f32)

    x = jax.random.normal(ks[0], (BATCH, SEQ, D_MODEL), f32)
    offset = jax.random.randint(ks[1], (BATCH, 1), 0, 1024, dtype=jnp.int32)
    positions = (offset + jnp.arange(SEQ, dtype=jnp.int32)[None, :]).astype(jnp.int32)
    dt0 = jnp.exp(jax.random.uniform(ks[10], (DEPTH, SSD_HEADS), f32,
                                     minval=math.log(1e-3), maxval=math.log(1e-1)))
    dt_bias = dt0 + jnp.log(-jnp.expm1(-dt0))
    a_log = jnp.log(jax.random.uniform(ks[11], (DEPTH, SSD_HEADS), f32, minval=1.0, maxval=16.0))
    return {
        'x': x,
        'positions': positions,
        'norm_ffn1': gain(ks[2], (DEPTH, D_MODEL)),
        'w_ffn1_in': dense(ks[3], (DEPTH, D_MODEL, 2 * D_FF), D_MODEL),
        'w_ffn1_out': dense(ks[4], (DEPTH, D_FF, D_MODEL), D_FF),
        'norm_mix': gain(ks[5], (DEPTH, D_MODEL)),
        'w_mix_in': dense(ks[6], (DEPTH, D_MODEL, D_IN_PROJ), D_MODEL),
        'conv_w': dense(ks[7], (DEPTH, SSD_CONV, CONV_DIM), SSD_CONV),
        'conv_b': 0.02 * jax.random.normal(ks[8], (DEPTH, CONV_DIM), f32),
        'dt_bias': dt_bias,
        'a_log': a_log,
        'd_skip': 1.0 + 0.1 * jax.random.normal(ks[9], (DEPTH, SSD_HEADS), f32),
        'ssd_norm': gain(ks[12], (DEPTH, D_SSD)),
        'w_mix_out': dense(ks[13], (DEPTH, D_MIX, D_MODEL), D_MIX),
        'norm_ffn2': gain(ks[14], (DEPTH, D_MODEL)),
        'w_ffn2_in': dense(ks[15], (DEPTH, D_MODEL, 2 * D_FF), D_MODEL),
        'w_ffn2_out': dense(ks[16], (DEPTH, D_FF, D_MODEL), D_FF),
        'final_norm': gain(ks[17], (D_MODEL,)),
    }


def reference(x, positions, norm_ffn1, w_ffn1_in, w_ffn1_out, norm_mix, w_mix_in,
              conv_w, conv_b, dt_bias, a_log, d_skip, ssd_norm, w_mix_out,
              norm_ffn2, w_ffn2_in, w_ffn2_out, final_norm):
    h = x
    for i in range(DEPTH):
        h = h + 0.5 * swiglu_ffn(rmsnorm(h, norm_ffn1[i]), w_ffn1_in[i], w_ffn1_out[i])
        u = rmsnorm(h, norm_mix[i]) @ w_mix_in[i]
        z, xbc, dt_raw, q, k, v, q_idx, k_idx, w_idx = jnp.split(u, SPLIT_POINTS, axis=-1)
        y_ssd = ssd_mixer(z, xbc, dt_raw, conv_w[i], conv_b[i], dt_bias[i], a_log[i],
                          d_skip[i], ssd_norm[i])
        y_att = dsa_mixer(q, k, v, q_idx, k_idx, w_idx, positions)
        h = h + jnp.concatenate([y_ssd, y_att], axis=-1) @ w_mix_out[i]
        h = h + 0.5 * swiglu_ffn(rmsnorm(h, norm_ffn2[i]), w_ffn2_in[i], w_ffn2_out[i])
    return rmsnorm(h, final_norm)
```

```python
import contextlib
import math
import numpy as np
import concourse.bass as bass
import concourse.mybir as mybir
from concourse.bass_utils import run_bass_kernel_spmd

F32 = mybir.dt.float32
BF16 = mybir.dt.bfloat16
I32 = mybir.dt.int32
AF = mybir.ActivationFunctionType
ALU = mybir.AluOpType
AX = mybir.AxisListType

D_MODEL = 1024
BATCH = 4
SEQ = 4096
DEPTH = 2
D_SSD = 1024
CONV_DIM = 1536
D_ATT = 1024
D_FF = 2816
D_IN_PROJ = 6232
EPS = 1e-6
ROPE_THETA = 500000.0
NCORES = 8
NEG = -1.0e30

O_Z, O_XBC, O_DT, O_Q, O_K, O_V, O_QI, O_KI, O_WI = 0, 1024, 2560, 2576, 3600, 4624, 5648, 6160, 6224


class Eng:
    def __init__(self, name, eng, sem, inc=1, pe=False):
        self.name, self.eng, self.sem, self.inc, self.pe = name, eng, sem, inc, pe
        self.count = 0
        self.waited = {}


class Tile:
    def __init__(self, ap, name):
        self.ap = ap
        self.name = name
        self.last_w = None
        self.readers = {}
        self.dmaeng = None

    def __getitem__(self, idx):
        return self.ap[idx]


class Ctx:
    def __init__(self, nc, st):
        self.nc, self.st = nc, st
        self.n_sem = 0
        self.PE = self._eng("pe", nc.tensor, pe=True)
        self.ACT = self._eng("act", nc.scalar)
        self.DVE = self._eng("dve", nc.vector)
        self.POOL = self._eng("pool", nc.gpsimd)
        self.SP = Eng("sp", nc.sync, None)
        self.uid = 0

    def _eng(self, name, eng, pe=False):
        sem = self.st.enter_context(self.nc.semaphore("sem_" + name))
        self.n_sem += 1
        return Eng(name, eng, sem, 1, pe)

    def sb(self, name, shape, dt):
        return Tile(self.st.enter_context(self.nc.sbuf_tensor(name, shape, dt))[:], name)

    def ps(self, name, shape, dt):
        return Tile(self.st.enter_context(self.nc.psum_tensor(name, shape, dt))[:], name)

    def sub(self, tile, idx, name=None):
        self.uid += 1
        return Tile(tile.ap[idx], name or (tile.name + "_s%d" % self.uid))

    def dram(self, name, shape, dt, kind="Internal"):
        return Tile(self.nc.dram_tensor(name, shape, dt, kind=kind).ap(), name)

    def _deps(self, E, reads, writes):
        deps = {}

        def add(e2, n, kind):
            if e2 is E and (E.pe or kind == 'war'):
                return
            if deps.get(e2.name, (None, 0))[1] < n:
                deps[e2.name] = (e2, n)
        for r in reads:
            if r.last_w is not None:
                add(r.last_w[0], r.last_w[1], 'raw')
        for w in writes:
            if w.last_w is not None:
                add(w.last_w[0], w.last_w[1], 'waw')
            for (e2, n) in w.readers.values():
                add(e2, n, 'war')
        for (e2, n) in deps.values():
            if E.waited.get(e2.name, 0) < n:
                E.eng.wait_ge(e2.sem, n * e2.inc)
                E.waited[e2.name] = n

    def op(self, E, fn, reads=(), writes=()):
        self._deps(E, reads, writes)
        inst = fn()
        E.count += 1
        inst.then_inc(E.sem, 1)
        for w in writes:
            w.last_w = (E, E.count)
            w.readers = {}
        for r in reads:
            r.readers[E.name] = (E, E.count)
        return inst

    def dma(self, Q, out, in_, reads=(), writes=(), **kw):
        self._deps(Q, reads, writes)
        inst = Q.eng.dma_start(out=out, in_=in_, **kw)
        owner = (list(writes) + list(reads))[0]
        if owner.dmaeng is None:
            self.uid += 1
            sem = self.st.enter_context(self.nc.semaphore("dsem%d" % self.uid))
            self.n_sem += 1
            owner.dmaeng = Eng("dma%d_%s" % (self.uid, owner.name), None, sem, 16)
        de = owner.dmaeng
        de.count += 1
        inst.then_inc(de.sem, 16)
        for w in writes:
            w.last_w = (de, de.count)
            w.readers = {}
        for r in reads:
            r.readers[de.name] = (de, de.count)
        return inst

    def finish(self, tiles):
        for t in tiles:
            self._deps(self.SP, [t], [t])


def rope_freqs(rot_dim):
    return [ROPE_THETA ** (-(2.0 * j) / rot_dim) for j in range(rot_dim // 2)]


PROJ_CHUNKS = [(0, 512, None), (512, 512, None), (1024, 512, None), (1536, 512, None), (2048, 512, None),
               (O_DT, 16, None),
               (O_Q, 512, (4, 128, 16, 0)), (O_Q + 512, 512, (4, 128, 16, 0)),
               (O_K, 512, (4, 128, 16, 0)), (O_K + 512, 512, (4, 128, 16, 0)),
               (O_V, 512, None), (O_V + 512, 512, None),
               (O_QI, 512, (8, 64, 8, 16)), (O_KI, 72, (1, 64, 8, 16))]
PROJ_SUBS = [(c0 + o, min(256, ncs - o)) for (c0, ncs, _) in PROJ_CHUNKS for o in range(0, ncs, 256)]
PROJ_SUB_IDX = {c0: i for i, (c0, n) in enumerate(PROJ_SUBS)}


def layout_ffn_wi(w):
    w = np.asarray(w, np.float32)
    g = w[:, :D_FF].reshape(8, 128, 22, 128)
    u = w[:, D_FF:].reshape(8, 128, 22, 128)
    gu = np.concatenate([g, u], -1)
    return np.ascontiguousarray(gu.transpose(2, 1, 0, 3)).reshape(22, 128, 2048)


def layout_wmi(w):
    w = np.asarray(w, np.float32)
    out = np.zeros((len(PROJ_SUBS), 128, 8, 256), np.float32)
    for i, (c0, n) in enumerate(PROJ_SUBS):
        out[i, :, :, :n] = w[:, c0:c0 + n].reshape(8, 128, n).transpose(1, 0, 2)
    return out.reshape(len(PROJ_SUBS), 128, 2048)


class TokProg:
    def __init__(self, ntok, stages):
        self.ntok = ntok
        self.nt = ntok // 128
        self.stages = stages
        self.nc = bass.Bass("TRN2", target_bir_lowering=False)
        self.build()

    def build(self):
        nc = self.nc
        nt = self.nt
        n_ffn = sum(1 for s in self.stages if s[0] == "ffn")
        has_proj = any(s[0] == "proj" for s in self.stages)
        has_mixout = any(s[0] == "mixout" for s in self.stages)
        has_final = any(s[0] == "final" for s in self.stages)
        h_in = nc.dram_tensor("h_in", [self.ntok, D_MODEL], F32, kind="ExternalInput").ap()
        dr = {}
        for i in range(n_ffn):
            dr["g%d" % i] = nc.dram_tensor("ffn_g%d" % i, [D_MODEL], F32, kind="ExternalInput").ap()
            dr["wi%d" % i] = nc.dram_tensor("ffn_wi%d" % i, [22, 128, 2048], F32, kind="ExternalInput").ap()
            dr["wo%d" % i] = nc.dram_tensor("ffn_wo%d" % i, [D_FF, D_MODEL], F32, kind="ExternalInput").ap()
        if has_mixout:
            dr["yT"] = nc.dram_tensor("yT", [2048, self.ntok], F32, kind="ExternalInput").ap()
            dr["wmo"] = nc.dram_tensor("wmo", [2048, D_MODEL], F32, kind="ExternalInput").ap()
        if has_proj:
            dr["gm"] = nc.dram_tensor("gm", [D_MODEL], F32, kind="ExternalInput").ap()
            dr["wmi"] = nc.dram_tensor("wmi", [len(PROJ_SUBS), 128, 2048], F32, kind="ExternalInput").ap()
            dr["pos"] = nc.dram_tensor("pos", [self.ntok], I32, kind="ExternalInput").ap()
            dr["u"] = nc.dram_tensor("u", [self.ntok, D_IN_PROJ], F32, kind="ExternalOutput").ap()
        if has_final:
            dr["gf"] = nc.dram_tensor("gf", [D_MODEL], F32, kind="ExternalInput").ap()
        h_out = nc.dram_tensor("h_out", [self.ntok, D_MODEL], F32, kind="ExternalOutput").ap()
        self.dr = dr

        with contextlib.ExitStack() as st:
            cx = Ctx(nc, st)
            self.cx = cx
            PE, ACT, DVE, POOL, SP = cx.PE, cx.ACT, cx.DVE, cx.POOL, cx.SP
            hbig = st.enter_context(nc.sbuf_tensor("hbig", [128, nt, D_MODEL], F32))
            h = [Tile(hbig[:, i, :], "h%d" % i) for i in range(nt)]
            self.h = h
            identf = cx.sb("identf", [128, 128], F32)
            ident = cx.sb("ident", [128, 128], BF16)
            cx.op(POOL, lambda: nc.gpsimd.memset(identf.ap[:], 0.0), writes=[identf])
            cx.op(POOL, lambda: nc.gpsimd.affine_select(out=identf.ap[:], in_=identf.ap[:], pattern=[[-1, 128]],
                                                         compare_op=ALU.not_equal, fill=1.0, base=0,
                                                         channel_multiplier=1), reads=[identf], writes=[identf])
            cx.op(POOL, lambda: nc.gpsimd.tensor_copy(out=ident.ap[:], in_=identf.ap[:]), reads=[identf], writes=[ident])
            self.ident = ident
            self.wbig = cx.sb("wbig", [128, 22, D_MODEL], BF16)
            self.wo_stage = [cx.sb("wo_st%d" % i, [128, D_MODEL], F32) for i in range(2)]
            self.wi_stage = [cx.sb("wi_st%d" % i, [128, 8, 256], F32) for i in range(2)]
            self.wi_bf = [cx.sb("wi_bf%d" % i, [128, 8, 256], BF16) for i in range(2)]
            self.gcol = cx.sb("gcol", [128, 8], F32)
            self.xnT = cx.sb("xnT", [128, 8, 512], BF16)
            self.actT = cx.sb("actT", [128, 22, 512], BF16)
            self.xn = [cx.sb("xn%d" % i, [128, D_MODEL], BF16) for i in range(2)]
            self.junk = cx.sb("junk", [128, D_MODEL], BF16)
            self.ss = [cx.sb("ss%d" % i, [128, 1], F32) for i in range(2)]
            self.rs = [cx.sb("rs%d" % i, [128, 1], F32) for i in range(2)]
            self.sg = [cx.sb("sg%d" % i, [128, 512], F32) for i in range(2)]
            self.tp = [cx.ps("tp%d" % i, [128, 8, 128], BF16) for i in range(2)]
            self.pg = [cx.ps("pg%d" % i, [128, 512], F32) for i in range(2)]
            self.pu = [cx.ps("pu%d" % i, [128, 512], F32) for i in range(2)]
            self.po = [cx.ps("po%d" % i, [128, 512], F32) for i in range(2)]
            self.wi_ctr = 0
            self.wo_ctr = 0
            self.tl_ctr = 0
            self.po_ctr = 0

            for i in range(nt):
                cx.dma(SP if i % 2 == 0 else POOL, h[i].ap, h_in[i * 128:(i + 1) * 128, :], writes=[h[i]])

            ffn_i = 0
            for s in self.stages:
                if s[0] == "ffn":
                    self.ffn(dr["g%d" % ffn_i], dr["wi%d" % ffn_i], dr["wo%d" % ffn_i])
                    ffn_i += 1
                elif s[0] == "mixout":
                    self.mixout()
                elif s[0] == "proj":
                    self.proj()
                elif s[0] == "final":
                    self.final()
            for i in range(nt):
                cx.dma(SP, h_out[i * 128:(i + 1) * 128, :], h[i].ap, reads=[h[i]])
            cx.finish(h + ([self.ust[0], self.ust[1]] if has_proj else []))

    def load_gcol(self, g_dram):
        cx = self.cx
        cx.dma(cx.SP, self.gcol.ap, g_dram.rearrange("(c p) -> p c", p=128), writes=[self.gcol],
               allow_slow_non_contiguous=True)

    def load_wbig(self, w_dram, nchunk):
        cx, nc = self.cx, self.nc
        for j in range(nchunk):
            stg = self.wo_stage[self.wo_ctr % 2]
            self.wo_ctr += 1
            cx.dma(cx.SP, stg.ap, w_dram[j * 128:(j + 1) * 128, :], writes=[stg])
            cx.op(cx.POOL, lambda: nc.gpsimd.tensor_copy(out=self.wbig.ap[:, j, :], in_=stg.ap[:]),
                  reads=[stg], writes=[self.wbig])

    def norm_group(self, t0, ntl):
        cx, nc = self.cx, self.nc
        for tl in range(ntl):
            k = self.tl_ctr % 2
            self.tl_ctr += 1
            hs, ss, rs, xn, tp = self.h[t0 + tl], self.ss[k], self.rs[k], self.xn[k], self.tp[k]
            cx.op(cx.ACT, lambda: nc.scalar.activation(out=self.junk.ap[:], in_=hs.ap, func=AF.Square, accum_out=ss.ap[:]),
                  reads=[hs], writes=[self.junk, ss])
            cx.op(cx.DVE, lambda: nc.vector.tensor_scalar(out=rs.ap[:], in0=ss.ap[:], scalar1=1.0 / D_MODEL, scalar2=EPS,
                                                         op0=ALU.mult, op1=ALU.add), reads=[ss], writes=[rs])
            cx.op(cx.ACT, lambda: nc.scalar.activation(out=rs.ap[:], in_=rs.ap[:], func=AF.Sqrt), reads=[rs], writes=[rs])
            cx.op(cx.DVE, lambda: nc.vector.reciprocal(out=rs.ap[:], in_=rs.ap[:]), reads=[rs], writes=[rs])
            cx.op(cx.DVE, lambda: nc.vector.tensor_scalar(out=xn.ap[:], in0=hs.ap, scalar1=rs.ap[:, 0:1], scalar2=None,
                                                         op0=ALU.mult), reads=[hs, rs], writes=[xn])
            for c in range(8):
                cx.op(cx.PE, lambda: nc.tensor.transpose(out=tp.ap[:, c, :], in_=xn.ap[:, c * 128:(c + 1) * 128],
                                                        identity=self.ident.ap[:]), reads=[xn, self.ident], writes=[tp])
            cx.op(cx.ACT, lambda: nc.scalar.copy(out=self.xnT.ap[:, :, tl * 128:(tl + 1) * 128], in_=tp.ap[:]),
                  reads=[tp], writes=[self.xnT])

    def ffn(self, g_dram, wi_dram, wo_dram):
        cx, nc = self.cx, self.nc
        PE, ACT, DVE, POOL, SP = cx.PE, cx.ACT, cx.DVE, cx.POOL, cx.SP
        self.load_gcol(g_dram)
        self.load_wbig(wo_dram, 22)
        ngrp = (self.nt + 3) // 4
        for gi in range(ngrp):
            t0 = gi * 4
            ntl = min(4, self.nt - t0)
            ncol = ntl * 128
            self.norm_group(t0, ntl)
            for j in range(22):
                k = self.wi_ctr % 2
                self.wi_ctr += 1
                stg, wbf = self.wi_stage[k], self.wi_bf[k]
                cx.dma(SP, stg.ap[:], wi_dram[j].rearrange("p (c n) -> p c n", c=8), writes=[stg])
                cx.op(POOL, lambda: nc.gpsimd.tensor_tensor(out=wbf.ap[:], in0=stg.ap[:],
                                                            in1=self.gcol.ap[:, :, None].to_broadcast([128, 8, 256]),
                                                            op=ALU.mult), reads=[stg, self.gcol], writes=[wbf])
                pg, pu, sg = self.pg[k], self.pu[k], self.sg[k]
                for c in range(8):
                    cx.op(PE, lambda: nc.tensor.matmul(pg.ap[:, 0:ncol], lhsT=wbf.ap[:, c, 0:128], rhs=self.xnT.ap[:, c, 0:ncol],
                                                       start=(c == 0), stop=(c == 7)), reads=[wbf, self.xnT], writes=[pg])
                for c in range(8):
                    cx.op(PE, lambda: nc.tensor.matmul(pu.ap[:, 0:ncol], lhsT=wbf.ap[:, c, 128:256], rhs=self.xnT.ap[:, c, 0:ncol],
                                                       start=(c == 0), stop=(c == 7)), reads=[wbf, self.xnT], writes=[pu])
                cx.op(ACT, lambda: nc.scalar.activation(out=sg.ap[:, 0:ncol], in_=pg.ap[:, 0:ncol], func=AF.Silu),
                      reads=[pg], writes=[sg])
                cx.op(DVE, lambda: nc.vector.tensor_tensor(out=self.actT.ap[:, j, 0:ncol], in0=sg.ap[:, 0:ncol],
                                                           in1=pu.ap[:, 0:ncol], op=ALU.mult),
                      reads=[sg, pu], writes=[self.actT])
            for tl in range(ntl):
                hs = self.h[t0 + tl]
                for dh in range(2):
                    po = self.po[self.po_ctr % 2]
                    self.po_ctr += 1
                    for j in range(22):
                        cx.op(PE, lambda: nc.tensor.matmul(po.ap[:], lhsT=self.actT.ap[:, j, tl * 128:(tl + 1) * 128],
                                                           rhs=self.wbig.ap[:, j, dh * 512:(dh + 1) * 512],
                                                           start=(j == 0), stop=(j == 21)),
                              reads=[self.actT, self.wbig], writes=[po])
                    cx.op(DVE, lambda: nc.vector.scalar_tensor_tensor(out=hs.ap[:, dh * 512:(dh + 1) * 512], in0=po.ap[:],
                                                                      scalar=0.5, in1=hs.ap[:, dh * 512:(dh + 1) * 512],
                                                                      op0=ALU.mult, op1=ALU.add),
                          reads=[po, hs], writes=[hs])

    def mixout(self):
        cx, nc = self.cx, self.nc
        PE, ACT, DVE, POOL, SP = cx.PE, cx.ACT, cx.DVE, cx.POOL, cx.SP
        self.load_wbig(self.dr["wmo"], 16)
        yT_v = self.dr["yT"].rearrange("(c p) t -> p c t", p=128)
        ngrp = (self.nt + 3) // 4
        for gi in range(ngrp):
            t0 = gi * 4
            ntl = min(4, self.nt - t0)
            ncol = ntl * 128
            for c in range(16):
                stg = self.wo_stage[self.wo_ctr % 2]
                self.wo_ctr += 1
                cx.dma(SP, stg.ap[:, 0:ncol], yT_v[:, c, t0 * 128:t0 * 128 + ncol], writes=[stg])
                cx.op(POOL, lambda: nc.gpsimd.tensor_copy(out=self.actT.ap[:, c, 0:ncol], in_=stg.ap[:, 0:ncol]),
                      reads=[stg], writes=[self.actT])
            for tl in range(ntl):
                hs = self.h[t0 + tl]
                for dh in range(2):
                    po = self.po[self.po_ctr % 2]
                    self.po_ctr += 1
                    for j in range(16):
                        cx.op(PE, lambda: nc.tensor.matmul(po.ap[:], lhsT=self.actT.ap[:, j, tl * 128:(tl + 1) * 128],
                                                           rhs=self.wbig.ap[:, j, dh * 512:(dh + 1) * 512],
                                                           start=(j == 0), stop=(j == 15)),
                              reads=[self.actT, self.wbig], writes=[po])
                    cx.op(DVE, lambda: nc.vector.tensor_tensor(out=hs.ap[:, dh * 512:(dh + 1) * 512], in0=po.ap[:],
                                                               in1=hs.ap[:, dh * 512:(dh + 1) * 512], op=ALU.add),
                          reads=[po, hs], writes=[hs])

    def rope_tables(self):
        cx, nc = self.cx, self.nc
        DVE, ACT, SP, POOL = cx.DVE, cx.ACT, cx.SP, cx.POOL
        nt = self.nt
        posi = cx.sb("posi", [128, nt], I32)
        posf = cx.sb("posf", [128, nt], F32)
        cx.dma(SP, posi.ap, self.dr["pos"].rearrange("(t p) -> p t", p=128), writes=[posi], allow_slow_non_contiguous=True)
        cx.op(DVE, lambda: nc.vector.tensor_copy(out=posf.ap[:], in_=posi.ap[:]), reads=[posi], writes=[posf])
        invf = cx.sb("invf", [128, 24], F32)
        fr = rope_freqs(32) + rope_freqs(16)
        for j, f in enumerate(fr):
            cx.op(DVE, lambda: nc.vector.memset(invf.ap[:, j:j + 1], float(f)), writes=[invf])
        ang = cx.sb("ang", [128, nt, 24], F32)
        red = cx.sb("red", [128, nt, 24], F32)
        ki = cx.sb("kint", [128, nt, 24], I32)
        kf = cx.sb("kflt", [128, nt, 24], F32)
        msk = cx.sb("rmsk", [128, nt, 24], F32)
        self.sin_t = cx.sb("sin_t", [128, nt, 24], F32)
        self.cos_t = cx.sb("cos_t", [128, nt, 24], F32)
        cx.op(DVE, lambda: nc.vector.tensor_tensor(out=ang.ap[:], in0=posf.ap[:, :, None].to_broadcast([128, nt, 24]),
                                                   in1=invf.ap[:, None, :].to_broadcast([128, nt, 24]), op=ALU.mult),
              reads=[posf, invf], writes=[ang])
        TWO_PI = 2.0 * math.pi
        C1 = 6.28125
        C2 = TWO_PI - C1
        for which, outt in ((0, self.sin_t), (1, self.cos_t)):
            src = ang
            if which == 1:
                cx.op(DVE, lambda: nc.vector.tensor_scalar(out=red.ap[:], in0=ang.ap[:], scalar1=math.pi / 2, scalar2=None,
                                                           op0=ALU.add), reads=[ang], writes=[red])
                src = red
            cx.op(DVE, lambda: nc.vector.tensor_scalar(out=kf.ap[:], in0=src.ap[:], scalar1=1.0 / TWO_PI, scalar2=None,
                                                       op0=ALU.mult), reads=[src], writes=[kf])
            cx.op(DVE, lambda: nc.vector.tensor_copy(out=ki.ap[:], in_=kf.ap[:]), reads=[kf], writes=[ki])
            cx.op(DVE, lambda: nc.vector.tensor_copy(out=kf.ap[:], in_=ki.ap[:]), reads=[ki], writes=[kf])
            cx.op(DVE, lambda: nc.vector.scalar_tensor_tensor(out=red.ap[:], in0=kf.ap[:], scalar=-C1, in1=src.ap[:],
                                                              op0=ALU.mult, op1=ALU.add), reads=[kf, src], writes=[red])
            cx.op(DVE, lambda: nc.vector.scalar_tensor_tensor(out=red.ap[:], in0=kf.ap[:], scalar=-C2, in1=red.ap[:],
                                                              op0=ALU.mult, op1=ALU.add), reads=[kf, red], writes=[red])
            cx.op(DVE, lambda: nc.vector.tensor_scalar(out=msk.ap[:], in0=red.ap[:], scalar1=math.pi, scalar2=-TWO_PI,
                                                       op0=ALU.is_gt, op1=ALU.mult), reads=[red], writes=[msk])
            cx.op(DVE, lambda: nc.vector.tensor_tensor(out=red.ap[:], in0=red.ap[:], in1=msk.ap[:], op=ALU.add),
                  reads=[red, msk], writes=[red])
            cx.op(DVE, lambda: nc.vector.tensor_scalar(out=msk.ap[:], in0=red.ap[:], scalar1=-math.pi, scalar2=TWO_PI,
                                                       op0=ALU.is_lt, op1=ALU.mult), reads=[red], writes=[msk])
            cx.op(DVE, lambda: nc.vector.tensor_tensor(out=red.ap[:], in0=red.ap[:], in1=msk.ap[:], op=ALU.add),
                  reads=[red, msk], writes=[red])
            cx.op(DVE, lambda: nc.vector.tensor_scalar(out=red.ap[:], in0=red.ap[:], scalar1=-math.pi, scalar2=math.pi,
                                                       op0=ALU.max, op1=ALU.min), reads=[red], writes=[red])
            cx.op(ACT, lambda: nc.scalar.activation(out=outt.ap[:], in_=red.ap[:], func=AF.Sin), reads=[red], writes=[outt])

    def rope_apply(self, X, H, D, half, tile_idx, tab_off):
        cx, nc = self.cx, self.nc
        DVE = cx.DVE
        xv = X.ap[:, 0:H * D].rearrange("p (h d) -> p h d", h=H)
        x1 = xv[:, :, 0:half]
        x2 = xv[:, :, half:2 * half]
        cosb = self.cos_t.ap[:, tile_idx:tile_idx + 1, tab_off:tab_off + half].to_broadcast([128, H, half])
        sinb = self.sin_t.ap[:, tile_idx:tile_idx + 1, tab_off:tab_off + half].to_broadcast([128, H, half])
        t = self.rtmp
        tv = [t.ap[:, i, 0:H * half].rearrange("p (h d) -> p h d", h=H) for i in range(4)]
        rd = [X, self.cos_t, self.sin_t]
        cx.op(DVE, lambda: nc.vector.tensor_tensor(out=tv[0], in0=x1, in1=cosb, op=ALU.mult), reads=rd, writes=[t])
        cx.op(DVE, lambda: nc.vector.tensor_tensor(out=tv[1], in0=x2, in1=sinb, op=ALU.mult), reads=rd, writes=[t])
        cx.op(DVE, lambda: nc.vector.tensor_tensor(out=tv[2], in0=x2, in1=cosb, op=ALU.mult), reads=rd, writes=[t])
        cx.op(DVE, lambda: nc.vector.tensor_tensor(out=tv[3], in0=x1, in1=sinb, op=ALU.mult), reads=rd, writes=[t])
        cx.op(DVE, lambda: nc.vector.tensor_tensor(out=x1, in0=tv[0], in1=tv[1], op=ALU.subtract), reads=[t], writes=[X])
        cx.op(DVE, lambda: nc.vector.tensor_tensor(out=x2, in0=tv[2], in1=tv[3], op=ALU.add), reads=[t], writes=[X])

    def proj(self):
        cx, nc = self.cx, self.nc
        PE, ACT, DVE, POOL, SP = cx.PE, cx.ACT, cx.DVE, cx.POOL, cx.SP
        self.rope_tables()
        self.rtmp = cx.sb("rtmp", [128, 4, 128], F32)
        self.ust = [cx.sb("ust%d" % i, [128, 512], F32) for i in range(2)]
        self.load_gcol(self.dr["gm"])
        chunks = PROJ_CHUNKS
        _unused = [(0, 512, None), (512, 512, None), (1024, 512, None), (1536, 512, None), (2048, 512, None),
                  (O_DT, 16, None),
                  (O_Q, 512, (4, 128, 16, 0)), (O_Q + 512, 512, (4, 128, 16, 0)),
                  (O_K, 512, (4, 128, 16, 0)), (O_K + 512, 512, (4, 128, 16, 0)),
                  (O_V, 512, None), (O_V + 512, 512, None),
                  (O_QI, 512, (8, 64, 8, 16)), (O_KI, 72, (1, 64, 8, 16))]
        u = self.dr["u"]
        ngrp = (self.nt + 3) // 4
        uc = 0
        for gi in range(ngrp):
            t0 = gi * 4
            ntl = min(4, self.nt - t0)
            self.norm_group(t0, ntl)
            for (c0, ncs, rp) in chunks:
                subs = [(c0 + o, min(256, ncs - o)) for o in range(0, ncs, 256)]
                wb = []
                for (sc0, sn) in subs:
                    k = self.wi_ctr % 2
                    self.wi_ctr += 1
                    stg, wbf = self.wi_stage[k], self.wi_bf[k]
                    cx.dma(SP, stg.ap[:], self.dr["wmi"][PROJ_SUB_IDX[sc0]].rearrange("p (c n) -> p c n", c=8), writes=[stg])
                    cx.op(POOL, lambda: nc.gpsimd.tensor_tensor(out=wbf.ap[:, :, 0:sn], in0=stg.ap[:, :, 0:sn],
                                                                in1=self.gcol.ap[:, :, None].to_broadcast([128, 8, sn]),
                                                                op=ALU.mult), reads=[stg, self.gcol], writes=[wbf])
                    wb.append((wbf, sn))
                for tl in range(ntl):
                    po = self.po[self.po_ctr % 2]
                    self.po_ctr += 1
                    o = 0
                    for (wbf, sn) in wb:
                        for c in range(8):
                            cx.op(PE, lambda: nc.tensor.matmul(po.ap[:, o:o + sn], lhsT=self.xnT.ap[:, c, tl * 128:(tl + 1) * 128],
                                                               rhs=wbf.ap[:, c, 0:sn], start=(c == 0), stop=(c == 7)),
                                  reads=[self.xnT, wbf], writes=[po])
                        o += sn
                    us = self.ust[uc % 2]
                    uc += 1
                    cx.op(ACT, lambda: nc.scalar.copy(out=us.ap[:, 0:ncs], in_=po.ap[:, 0:ncs]), reads=[po], writes=[us])
                    if rp is not None:
                        H, D, half, toff = rp
                        self.rope_apply(us, H, D, half, t0 + tl, toff)
                    cx.dma(SP, u[(t0 + tl) * 128:(t0 + tl + 1) * 128, c0:c0 + ncs], us.ap[:, 0:ncs], reads=[us])

    def final(self):
        cx, nc = self.cx, self.nc
        PE, ACT, DVE, POOL, SP = cx.PE, cx.ACT, cx.DVE, cx.POOL, cx.SP
        gb = cx.sb("gfb", [128, D_MODEL], F32)
        cx.dma(SP, gb.ap, self.dr["gf"].partition_broadcast(128), writes=[gb])
        for i in range(self.nt):
            k = self.tl_ctr % 2
            self.tl_ctr += 1
            hs, ss, rs = self.h[i], self.ss[k], self.rs[k]
            cx.op(ACT, lambda: nc.scalar.activation(out=self.junk.ap[:], in_=hs.ap, func=AF.Square, accum_out=ss.ap[:]),
                  reads=[hs], writes=[self.junk, ss])
            cx.op(DVE, lambda: nc.vector.tensor_scalar(out=rs.ap[:], in0=ss.ap[:], scalar1=1.0 / D_MODEL, scalar2=EPS,
                                                       op0=ALU.mult, op1=ALU.add), reads=[ss], writes=[rs])
            cx.op(ACT, lambda: nc.scalar.activation(out=rs.ap[:], in_=rs.ap[:], func=AF.Sqrt), reads=[rs], writes=[rs])
            cx.op(DVE, lambda: nc.vector.reciprocal(out=rs.ap[:], in_=rs.ap[:]), reads=[rs], writes=[rs])
            cx.op(DVE, lambda: nc.vector.scalar_tensor_tensor(out=hs.ap, in0=hs.ap, scalar=rs.ap[:, 0:1], in1=gb.ap[:],
                                                              op0=ALU.mult, op1=ALU.mult), reads=[hs, rs, gb], writes=[hs])


class SsdProg:
    def __init__(self, L=SEQ):
        self.L = L
        self.nt = L // 128
        self.nc = bass.Bass("TRN2", target_bir_lowering=False)
        self.build()

    def build(self):
        nc, L, nt = self.nc, self.L, self.nt
        xbcT = nc.dram_tensor("xbcT", [768, L], F32, kind="ExternalInput").ap()
        cw_d = nc.dram_tensor("cw", [768, 4], F32, kind="ExternalInput").ap()
        cb_d = nc.dram_tensor("cb", [768], F32, kind="ExternalInput").ap()
        z_d = nc.dram_tensor("z", [L, 512], F32, kind="ExternalInput").ap()
        dtr_d = nc.dram_tensor("dtr", [L, 8], F32, kind="ExternalInput").ap()
        dtb_d = nc.dram_tensor("dtb", [8], F32, kind="ExternalInput").ap()
        alog_d = nc.dram_tensor("alog", [8], F32, kind="ExternalInput").ap()
        dsk_d = nc.dram_tensor("dsk", [8], F32, kind="ExternalInput").ap()
        ng_d = nc.dram_tensor("ng", [512], F32, kind="ExternalInput").ap()
        y_d = nc.dram_tensor("y", [L, 512], F32, kind="ExternalOutput").ap()
        with contextlib.ExitStack() as st:
            cx = Ctx(nc, st)
            self.cx = cx
            PE, ACT, DVE, POOL, SP = cx.PE, cx.ACT, cx.DVE, cx.POOL, cx.SP
            identf = cx.sb("identf", [128, 128], F32)
            identb = cx.sb("identb", [128, 128], BF16)
            ones = cx.sb("ones", [128, 128], F32)
            U = cx.sb("U", [128, 128], F32)
            A = cx.sb("A", [128, 128], F32)
            cx.op(POOL, lambda: nc.gpsimd.memset(ones.ap[:], 1.0), writes=[ones])
            cx.op(POOL, lambda: nc.gpsimd.memset(identf.ap[:], 0.0), writes=[identf])
            cx.op(POOL, lambda: nc.gpsimd.affine_select(out=identf.ap[:], in_=identf.ap[:], pattern=[[-1, 128]],
                                                         compare_op=ALU.not_equal, fill=1.0, base=0,
                                                         channel_multiplier=1), reads=[identf], writes=[identf])
            cx.op(POOL, lambda: nc.gpsimd.tensor_copy(out=identb.ap[:], in_=identf.ap[:]), reads=[identf], writes=[identb])
            cx.op(POOL, lambda: nc.gpsimd.affine_select(out=U.ap[:], in_=ones.ap[:], pattern=[[1, 128]],
                                                         compare_op=ALU.is_ge, fill=0.0, base=0,
                                                         channel_multiplier=-1), reads=[ones], writes=[U])
            cx.op(POOL, lambda: nc.gpsimd.affine_select(out=A.ap[:], in_=ones.ap[:], pattern=[[-1, 128]],
                                                         compare_op=ALU.is_gt, fill=0.0, base=0,
                                                         channel_multiplier=1), reads=[ones], writes=[A])
            cw = cx.sb("cw_sb", [128, 6, 4], F32)
            cbs = cx.sb("cbs", [128, 6], F32)
            cx.dma(SP, cw.ap, cw_d.rearrange("(c p) k -> p c k", p=128), writes=[cw])
            cx.dma(SP, cbs.ap, cb_d.rearrange("(c p) -> p c", p=128), writes=[cbs], allow_slow_non_contiguous=True)
            dtb = cx.sb("dtb_sb", [128, 8], F32)
            alog = cx.sb("alog_sb", [128, 8], F32)
            dsk = cx.sb("dsk_sb", [128, 8], F32)
            ngb = cx.sb("ngb", [128, 512], F32)
            cx.dma(SP, dtb.ap, dtb_d.partition_broadcast(128), writes=[dtb])
            cx.dma(SP, alog.ap, alog_d.partition_broadcast(128), writes=[alog])
            cx.dma(SP, dsk.ap, dsk_d.partition_broadcast(128), writes=[dsk])
            cx.dma(SP, ngb.ap, ng_d.partition_broadcast(128), writes=[ngb])
            aneg = cx.sb("aneg", [128, 8], F32)
            cx.op(ACT, lambda: nc.scalar.activation(out=aneg.ap[:], in_=alog.ap[:], func=AF.Exp), reads=[alog], writes=[aneg])
            cx.op(DVE, lambda: nc.vector.tensor_scalar(out=aneg.ap[:], in0=aneg.ap[:], scalar1=-1.0, scalar2=None, op0=ALU.mult),
                  reads=[aneg], writes=[aneg])
            dt_all = cx.sb("dt_all", [128, nt, 8], F32)
            adt_all = cx.sb("adt_all", [128, nt, 8], F32)
            t1 = cx.sb("sp_t1", [128, nt, 8], F32)
            t2 = cx.sb("sp_t2", [128, nt, 8], F32)
            cx.dma(SP, dt_all.ap, dtr_d.rearrange("(t p) h -> p t h", p=128), writes=[dt_all])
            cx.op(DVE, lambda: nc.vector.tensor_tensor(out=dt_all.ap[:], in0=dt_all.ap[:],
                                                       in1=dtb.ap[:, None, :].to_broadcast([128, nt, 8]), op=ALU.add),
                  reads=[dt_all, dtb], writes=[dt_all])
            cx.op(ACT, lambda: nc.scalar.activation(out=t1.ap[:], in_=dt_all.ap[:], func=AF.Abs), reads=[dt_all], writes=[t1])
            cx.op(ACT, lambda: nc.scalar.activation(out=t1.ap[:], in_=t1.ap[:], func=AF.Exp, scale=-1.0), reads=[t1], writes=[t1])
            cx.op(DVE, lambda: nc.vector.tensor_scalar(out=t1.ap[:], in0=t1.ap[:], scalar1=1.0, scalar2=None, op0=ALU.add),
                  reads=[t1], writes=[t1])
            cx.op(ACT, lambda: nc.scalar.activation(out=t2.ap[:], in_=t1.ap[:], func=AF.Ln), reads=[t1], writes=[t2])
            cx.op(DVE, lambda: nc.vector.tensor_scalar(out=dt_all.ap[:], in0=dt_all.ap[:], scalar1=0.0, scalar2=None, op0=ALU.max),
                  reads=[dt_all], writes=[dt_all])
            cx.op(DVE, lambda: nc.vector.tensor_tensor(out=dt_all.ap[:], in0=dt_all.ap[:], in1=t2.ap[:], op=ALU.add),
                  reads=[dt_all, t2], writes=[dt_all])
            cx.op(DVE, lambda: nc.vector.tensor_tensor(out=adt_all.ap[:], in0=dt_all.ap[:],
                                                       in1=aneg.ap[:, None, :].to_broadcast([128, nt, 8]), op=ALU.mult),
                  reads=[dt_all, aneg], writes=[adt_all])
            pb = [cx.ps("pb%d" % i, [128, 512], F32) for i in range(6)]
            pdiff = cx.ps("pdiff", [128, 1024], F32)
            x_tok = cx.sb("x_tok", [128, nt, 512], F32)
            BT = cx.sb("BT", [128, L], BF16)
            CT = cx.sb("CT", [128, L], BF16)
            B_tok = cx.sb("B_tok", [128, nt, 128], BF16)
            HL = min(2048, L)
            nhalf = L // HL
            xpad = [cx.sb("xpad%d" % i, [128, 3 + HL], F32) for i in range(2)]
            cacc = cx.sb("cacc", [128, HL], F32)
            cso = cx.sb("cso", [128, HL], F32)
            ctr = 0
            tpc = 0
            for hh in range(nhalf):
                for cc in range(6):
                    xp = xpad[ctr % 2]
                    ctr += 1
                    if hh == 0:
                        cx.op(POOL, lambda: nc.gpsimd.memset(xp.ap[:, 0:3], 0.0), writes=[xp])
                        cx.dma(SP, xp.ap[:, 3:3 + HL], xbcT[cc * 128:(cc + 1) * 128, 0:HL], writes=[xp])
                    else:
                        cx.dma(SP, xp.ap[:, 0:3 + HL], xbcT[cc * 128:(cc + 1) * 128, hh * HL - 3:(hh + 1) * HL], writes=[xp])
                    cx.op(DVE, lambda: nc.vector.tensor_scalar(out=cacc.ap[:], in0=xp.ap[:, 3:3 + HL], scalar1=cw.ap[:, cc, 3:4],
                                                               scalar2=cbs.ap[:, cc:cc + 1], op0=ALU.mult, op1=ALU.add),
                          reads=[xp, cw, cbs], writes=[cacc])
                    for k in (2, 1, 0):
                        cx.op(DVE, lambda: nc.vector.scalar_tensor_tensor(out=cacc.ap[:], in0=xp.ap[:, k:k + HL],
                                                                          scalar=cw.ap[:, cc, k:k + 1], in1=cacc.ap[:],
                                                                          op0=ALU.mult, op1=ALU.add),
                              reads=[xp, cw, cacc], writes=[cacc])
                    if cc < 4:
                        cx.op(ACT, lambda: nc.scalar.activation(out=cso.ap[:], in_=cacc.ap[:], func=AF.Silu),
                              reads=[cacc], writes=[cso])
                        for t4 in range(HL // 512):
                            pt = pb[tpc % 2]
                            tpc += 1
                            for q in range(4):
                                col = t4 * 512 + q * 128
                                cx.op(PE, lambda: nc.tensor.transpose(out=pt.ap[:, q * 128:(q + 1) * 128],
                                                                      in_=cso.ap[:, col:col + 128], identity=identf.ap[:]),
                                      reads=[cso, identf], writes=[pt])
                            tt0 = (hh * HL) // 128 + t4 * 4
                            cx.op(ACT, lambda: nc.scalar.copy(out=x_tok.ap[:, tt0:tt0 + 4, cc * 128:(cc + 1) * 128],
                                                              in_=pt.ap[:].rearrange("p (q c) -> p q c", q=4)),
                                  reads=[pt], writes=[x_tok])
                    elif cc == 4:
                        cx.op(ACT, lambda: nc.scalar.activation(out=BT.ap[:, hh * HL:(hh + 1) * HL], in_=cacc.ap[:], func=AF.Silu),
                              reads=[cacc], writes=[BT])
                        for t4 in range(HL // 512):
                            pt = pb[tpc % 2]
                            tpc += 1
                            ptb = pt.ap[:].bitcast(BF16)
                            for q in range(4):
                                col = hh * HL + t4 * 512 + q * 128
                                cx.op(PE, lambda: nc.tensor.transpose(out=ptb[:, q * 128:(q + 1) * 128],
                                                                      in_=BT.ap[:, col:col + 128], identity=identb.ap[:]),
                                      reads=[BT, identb], writes=[pt])
                            tt0 = (hh * HL) // 128 + t4 * 4
                            cx.op(ACT, lambda: nc.scalar.copy(out=B_tok.ap[:, tt0:tt0 + 4, :],
                                                              in_=ptb[:, 0:512].rearrange("p (q c) -> p q c", q=4)),
                                  reads=[pt], writes=[B_tok])
                    else:
                        cx.op(ACT, lambda: nc.scalar.activation(out=CT.ap[:, hh * HL:(hh + 1) * HL], in_=cacc.ap[:], func=AF.Silu),
                              reads=[cacc], writes=[CT])
            Hs = cx.sb("Hs", [128, 512], F32)
            Hbf = cx.sb("Hbf", [128, 512], BF16)
            LA = [cx.sb("LA%d" % i, [128, 8, 128], F32) for i in range(2)]
            LO = cx.sb("LO", [128, 8, 128], F32)
            dec = cx.sb("dec", [128, 8, 128], F32)
            cbm = cx.sb("cbm", [128, 128], F32)
            MT = [cx.sb("MT%d" % i, [128, 8, 128], BF16) for i in range(3)]
            xdt = [cx.sb("xdt%d" % i, [128, 512], BF16) for i in range(2)]
            xdtw = [cx.sb("xdtw%d" % i, [128, 512], BF16) for i in range(2)]
            sm = cx.sb("sm", [128, 5, 8], F32)
            zt = [cx.sb("zt%d" % i, [128, 512], F32) for i in range(2)]
            ya = [cx.sb("ya%d" % i, [128, 512], F32) for i in range(2)]
            yb = cx.sb("yb", [128, 512], F32)
            sq = cx.sb("sq", [128, 512], F32)
            ss = cx.sb("ss", [128, 1], F32)
            p_cb, p_yd, p_yo, p_S, p_sm = pb[0], pb[1], pb[2], pb[3], pb[4]
            nchunk = nt // 2
            zc = 0
            for c in range(nchunk):
                tl = [2 * c, 2 * c + 1]
                adt = [adt_all.ap[:, t, :] for t in tl]
                smv = p_sm.ap[:, 0:40].rearrange("p (a h) -> p a h", a=5)
                rd = [adt_all, U, A, ones]
                cx.op(PE, lambda: nc.tensor.matmul(smv[:, 0, :], lhsT=U.ap[:], rhs=adt[0], start=True, stop=True), reads=rd, writes=[p_sm])
                cx.op(PE, lambda: nc.tensor.matmul(smv[:, 1, :], lhsT=U.ap[:], rhs=adt[1], start=True, stop=False), reads=rd, writes=[p_sm])
                cx.op(PE, lambda: nc.tensor.matmul(smv[:, 1, :], lhsT=ones.ap[:], rhs=adt[0], start=False, stop=True), reads=rd, writes=[p_sm])
                cx.op(PE, lambda: nc.tensor.matmul(smv[:, 2, :], lhsT=A.ap[:], rhs=adt[0], start=True, stop=False), reads=rd, writes=[p_sm])
                cx.op(PE, lambda: nc.tensor.matmul(smv[:, 2, :], lhsT=ones.ap[:], rhs=adt[1], start=False, stop=True), reads=rd, writes=[p_sm])
                cx.op(PE, lambda: nc.tensor.matmul(smv[:, 3, :], lhsT=A.ap[:], rhs=adt[1], start=True, stop=True), reads=rd, writes=[p_sm])
                cx.op(PE, lambda: nc.tensor.matmul(smv[:, 4, :], lhsT=ones.ap[:], rhs=adt[0], start=True, stop=False), reads=rd, writes=[p_sm])
                cx.op(PE, lambda: nc.tensor.matmul(smv[:, 4, :], lhsT=ones.ap[:], rhs=adt[1], start=False, stop=True), reads=rd, writes=[p_sm])
                cx.op(ACT, lambda: nc.scalar.activation(out=sm.ap[:], in_=smv, func=AF.Exp), reads=[p_sm], writes=[sm])
                for i in range(2):
                    cx.op(DVE, lambda: nc.vector.tensor_tensor(out=LA[i].ap[:], in0=A.ap[:, None, :].to_broadcast([128, 8, 128]),
                                                               in1=adt[i][:, :, None].to_broadcast([128, 8, 128]), op=ALU.mult),
                          reads=[A, adt_all], writes=[LA[i]])
                cx.op(DVE, lambda: nc.vector.tensor_tensor(out=LO.ap[:], in0=ones.ap[:, None, :].to_broadcast([128, 8, 128]),
                                                           in1=adt[1][:, :, None].to_broadcast([128, 8, 128]), op=ALU.mult),
                      reads=[ones, adt_all], writes=[LO])
                for i in range(2):
                    t = tl[i]
                    cx.op(DVE, lambda: nc.vector.tensor_tensor(out=xdt[i].ap[:].rearrange("p (h d) -> p h d", h=8),
                                                               in0=x_tok.ap[:, t, :].rearrange("p (h d) -> p h d", h=8),
                                                               in1=dt_all.ap[:, t, :, None].to_broadcast([128, 8, 64]), op=ALU.mult),
                          reads=[x_tok, dt_all], writes=[xdt[i]])
                    cx.op(DVE, lambda: nc.vector.tensor_tensor(out=xdtw[i].ap[:].rearrange("p (h d) -> p h d", h=8),
                                                               in0=xdt[i].ap[:].rearrange("p (h d) -> p h d", h=8),
                                                               in1=sm.ap[:, 2 + i, :, None].to_broadcast([128, 8, 64]), op=ALU.mult),
                          reads=[xdt[i], sm], writes=[xdtw[i]])
                for bi, (sg, lm) in enumerate([(0, 0), (1, 1), (0, 1)]):
                    scol, lcol = tl[sg] * 128, tl[lm] * 128
                    for hd in range(8):
                        o = pdiff.ap[:, hd * 128:(hd + 1) * 128]
                        if sg == lm:
                            cx.op(PE, lambda: nc.tensor.matmul(o, lhsT=LA[sg].ap[:, hd, :], rhs=U.ap[:], start=True, stop=True),
                                  reads=[LA[sg], U], writes=[pdiff])
                        else:
                            cx.op(PE, lambda: nc.tensor.matmul(o, lhsT=LA[0].ap[:, hd, :], rhs=ones.ap[:], start=True, stop=False),
                                  reads=[LA[0], ones], writes=[pdiff])
                            cx.op(PE, lambda: nc.tensor.matmul(o, lhsT=LO.ap[:, hd, :], rhs=U.ap[:], start=False, stop=True),
                                  reads=[LO, U], writes=[pdiff])
                    cx.op(PE, lambda: nc.tensor.matmul(p_cb.ap[:, 0:128], lhsT=BT.ap[:, scol:scol + 128], rhs=CT.ap[:, lcol:lcol + 128],
                                                       start=True, stop=True), reads=[BT, CT], writes=[p_cb])
                    cx.op(ACT, lambda: nc.scalar.activation(out=dec.ap[:].rearrange("p h l -> p (h l)"), in_=pdiff.ap[:], func=AF.Exp),
                          reads=[pdiff], writes=[dec])
                    if sg == lm:
                        cx.op(DVE, lambda: nc.vector.tensor_tensor(out=cbm.ap[:], in0=p_cb.ap[:, 0:128], in1=U.ap[:], op=ALU.mult),
                              reads=[p_cb, U], writes=[cbm])
                    else:
                        cx.op(DVE, lambda: nc.vector.tensor_copy(out=cbm.ap[:], in_=p_cb.ap[:, 0:128]), reads=[p_cb], writes=[cbm])
                    cx.op(DVE, lambda: nc.vector.tensor_tensor(out=MT[bi].ap[:], in0=dec.ap[:],
                                                               in1=cbm.ap[:, None, :].to_broadcast([128, 8, 128]), op=ALU.mult),
                          reads=[dec, cbm], writes=[MT[bi]])
                for lm in range(2):
                    t = tl[lm]
                    lcol = t * 128
                    for hd in range(8):
                        o = p_yd.ap[:, hd * 64:(hd + 1) * 64]
                        if lm == 0:
                            cx.op(PE, lambda: nc.tensor.matmul(o, lhsT=MT[0].ap[:, hd, :], rhs=xdt[0].ap[:, hd * 64:(hd + 1) * 64],
                                                               start=True, stop=True), reads=[MT[0], xdt[0]], writes=[p_yd])
                        else:
                            cx.op(PE, lambda: nc.tensor.matmul(o, lhsT=MT[2].ap[:, hd, :], rhs=xdt[0].ap[:, hd * 64:(hd + 1) * 64],
                                                               start=True, stop=False), reads=[MT[2], xdt[0]], writes=[p_yd])
                            cx.op(PE, lambda: nc.tensor.matmul(o, lhsT=MT[1].ap[:, hd, :], rhs=xdt[1].ap[:, hd * 64:(hd + 1) * 64],
                                                               start=False, stop=True), reads=[MT[1], xdt[1]], writes=[p_yd])
                    y1 = ya[lm]
                    if c > 0:
                        cx.op(PE, lambda: nc.tensor.matmul(p_yo.ap[:], lhsT=CT.ap[:, lcol:lcol + 128], rhs=Hbf.ap[:], start=True, stop=True),
                              reads=[CT, Hbf], writes=[p_yo])
                        cx.op(DVE, lambda: nc.vector.tensor_tensor(out=yb.ap[:].rearrange("p (h d) -> p h d", h=8),
                                                                   in0=p_yo.ap[:].rearrange("p (h d) -> p h d", h=8),
                                                                   in1=sm.ap[:, lm, :, None].to_broadcast([128, 8, 64]), op=ALU.mult),
                              reads=[p_yo, sm], writes=[yb])
                        cx.op(DVE, lambda: nc.vector.tensor_tensor(out=y1.ap[:], in0=p_yd.ap[:], in1=yb.ap[:], op=ALU.add),
                              reads=[p_yd, yb], writes=[y1])
                    else:
                        cx.op(DVE, lambda: nc.vector.tensor_copy(out=y1.ap[:], in_=p_yd.ap[:]), reads=[p_yd], writes=[y1])
                    cx.op(DVE, lambda: nc.vector.tensor_tensor(out=yb.ap[:].rearrange("p (h d) -> p h d", h=8),
                                                               in0=x_tok.ap[:, t, :].rearrange("p (h d) -> p h d", h=8),
                                                               in1=dsk.ap[:, :, None].to_broadcast([128, 8, 64]), op=ALU.mult),
                          reads=[x_tok, dsk], writes=[yb])
                    cx.op(DVE, lambda: nc.vector.tensor_tensor(out=y1.ap[:], in0=y1.ap[:], in1=yb.ap[:], op=ALU.add),
                          reads=[y1, yb], writes=[y1])
                    zz = zt[zc % 2]
                    zc += 1
                    cx.dma(SP, zz.ap, z_d[t * 128:(t + 1) * 128, :], writes=[zz])
                    cx.op(ACT, lambda: nc.scalar.activation(out=zz.ap[:], in_=zz.ap[:], func=AF.Silu), reads=[zz], writes=[zz])
                    cx.op(DVE, lambda: nc.vector.tensor_tensor(out=y1.ap[:], in0=y1.ap[:], in1=zz.ap[:], op=ALU.mult),
                          reads=[y1, zz], writes=[y1])
                    cx.op(ACT, lambda: nc.scalar.activation(out=sq.ap[:], in_=y1.ap[:], func=AF.Square, accum_out=ss.ap[:]),
                          reads=[y1], writes=[sq, ss])
                    cx.op(DVE, lambda: nc.vector.tensor_scalar(out=ss.ap[:], in0=ss.ap[:], scalar1=1.0 / 512, scalar2=EPS,
                                                               op0=ALU.mult, op1=ALU.add), reads=[ss], writes=[ss])
                    cx.op(ACT, lambda: nc.scalar.activation(out=ss.ap[:], in_=ss.ap[:], func=AF.Sqrt), reads=[ss], writes=[ss])
                    cx.op(DVE, lambda: nc.vector.reciprocal(out=ss.ap[:], in_=ss.ap[:]), reads=[ss], writes=[ss])
                    cx.op(DVE, lambda: nc.vector.scalar_tensor_tensor(out=y1.ap[:], in0=y1.ap[:], scalar=ss.ap[:, 0:1], in1=ngb.ap[:],
                                                                      op0=ALU.mult, op1=ALU.mult), reads=[y1, ss, ngb], writes=[y1])
                    cx.dma(SP, y_d[t * 128:(t + 1) * 128, :], y1.ap[:], reads=[y1])
                if c < nchunk - 1:
                    for i in range(2):
                        cx.op(PE, lambda: nc.tensor.matmul(p_S.ap[:], lhsT=B_tok.ap[:, tl[i], :], rhs=xdtw[i].ap[:], start=(i == 0), stop=(i == 1)),
                              reads=[B_tok, xdtw[i]], writes=[p_S])
                    if c == 0:
                        cx.op(DVE, lambda: nc.vector.tensor_copy(out=Hs.ap[:], in_=p_S.ap[:]), reads=[p_S], writes=[Hs])
                    else:
                        cx.op(DVE, lambda: nc.vector.tensor_tensor(out=Hs.ap[:].rearrange("p (h d) -> p h d", h=8),
                                                                   in0=Hs.ap[:].rearrange("p (h d) -> p h d", h=8),
                                                                   in1=sm.ap[:, 4, :, None].to_broadcast([128, 8, 64]), op=ALU.mult),
                              reads=[Hs, sm], writes=[Hs])
                        cx.op(DVE, lambda: nc.vector.tensor_tensor(out=Hs.ap[:], in0=Hs.ap[:], in1=p_S.ap[:], op=ALU.add),
                              reads=[Hs, p_S], writes=[Hs])
                    cx.op(ACT, lambda: nc.scalar.copy(out=Hbf.ap[:], in_=Hs.ap[:]), reads=[Hs], writes=[Hbf])
            cx.finish(ya)


RNEG = -3.0e38
NBISECT = 22
IDX_SCALE = (8 ** -0.5) * (64 ** -0.5)
ATT_SCALE = 128 ** -0.5


class DsaProg:
    def __init__(self, L=SEQ):
        self.L = L
        self.nqb = L // 256
        self.NQ = self.nqb * 128
        self.nc = bass.Bass("TRN2", target_bir_lowering=False)
        self.build()

    def build(self):
        nc, L, nqb, NQ = self.nc, self.L, self.nqb, self.NQ
        nt = L // 128
        qT_d = nc.dram_tensor("qT", [1024, NQ], F32, kind="ExternalInput").ap()
        kT_d = nc.dram_tensor("kT", [1024, L], F32, kind="ExternalInput").ap()
        v_d = nc.dram_tensor("v", [L, 1024], F32, kind="ExternalInput").ap()
        qiT_d = nc.dram_tensor("qiT", [512, NQ], F32, kind="ExternalInput").ap()
        kiT_d = nc.dram_tensor("kiT", [64, L], F32, kind="ExternalInput").ap()
        wi_d = nc.dram_tensor("wi", [NQ, 8], F32, kind="ExternalInput").ap()
        mk_d = nc.dram_tensor("cmask", [128, 256], F32, kind="ExternalInput").ap()
        o_d = nc.dram_tensor("o", [NQ, 1024], F32, kind="ExternalOutput").ap()
        with contextlib.ExitStack() as st:
            cx = Ctx(nc, st)
            self.cx = cx
            PE, ACT, DVE, POOL, SP = cx.PE, cx.ACT, cx.DVE, cx.POOL, cx.SP
            identf = cx.sb("identf", [128, 128], F32)
            identb = cx.sb("identb", [128, 128], BF16)
            cx.op(POOL, lambda: nc.gpsimd.memset(identf.ap[:], 0.0), writes=[identf])
            cx.op(POOL, lambda: nc.gpsimd.affine_select(out=identf.ap[:], in_=identf.ap[:], pattern=[[-1, 128]],
                                                         compare_op=ALU.not_equal, fill=1.0, base=0,
                                                         channel_multiplier=1), reads=[identf], writes=[identf])
            cx.op(POOL, lambda: nc.gpsimd.tensor_copy(out=identb.ap[:], in_=identf.ap[:]), reads=[identf], writes=[identb])
            KT = cx.sb("KT", [128, 8, L], BF16)
            V = cx.sb("V", [128, nt, 1024], BF16)
            kiT = cx.sb("kiT_sb", [64, L], BF16)
            stage = [cx.sb("stage%d" % i, [128, 512], F32) for i in range(2)]
            sc_ = 0
            for c0 in range(0, L, 512):
                stg = stage[sc_ % 2]
                sc_ += 1
                cx.dma(SP, stg.ap[0:64, :], kiT_d[:, c0:c0 + 512], writes=[stg])
                cx.op(POOL, lambda: nc.gpsimd.tensor_copy(out=kiT.ap[:, c0:c0 + 512], in_=stg.ap[0:64, :]), reads=[stg], writes=[kiT])
            for hd in range(8):
                for c0 in range(0, L, 512):
                    stg = stage[sc_ % 2]
                    sc_ += 1
                    cx.dma(SP, stg.ap[:], kT_d[hd * 128:(hd + 1) * 128, c0:c0 + 512], writes=[stg])
                    cx.op(POOL, lambda: nc.gpsimd.tensor_copy(out=KT.ap[:, hd, c0:c0 + 512], in_=stg.ap[:]), reads=[stg], writes=[KT])
            for t in range(nt):
                for c0 in (0, 512):
                    stg = stage[sc_ % 2]
                    sc_ += 1
                    cx.dma(SP, stg.ap[:], v_d[t * 128:(t + 1) * 128, c0:c0 + 512], writes=[stg])
                    cx.op(POOL, lambda: nc.gpsimd.tensor_copy(out=V.ap[:, t, c0:c0 + 512], in_=stg.ap[:]), reads=[stg], writes=[V])
            wsc = cx.sb("wsc", [128, nqb, 8], F32)
            cx.dma(SP, wsc.ap, wi_d.rearrange("(i p) h -> p i h", p=128), writes=[wsc])
            cx.op(DVE, lambda: nc.vector.tensor_scalar(out=wsc.ap[:], in0=wsc.ap[:], scalar1=IDX_SCALE, scalar2=None, op0=ALU.mult),
                  reads=[wsc], writes=[wsc])
            cmask = cx.sb("cmask_sb", [128, 256], F32)
            cx.dma(SP, cmask.ap, mk_d, writes=[cmask])
            thr0 = cx.sb("thr0", [128, 1], F32)
            cx.op(DVE, lambda: nc.vector.memset(thr0.ap[:], NEG / 2), writes=[thr0])
            score = cx.sb("score", [128, L], F32)
            work = cx.sb("work", [128, L], F32)
            mb = cx.sb("mb", [128, L], BF16)
            rl = [cx.sb("rl%d" % i, [128, 512], F32) for i in range(2)]
            qst = cx.sb("qst", [128, 8, 128], F32)
            qb = [cx.sb("qb%d" % i, [128, 8, 128], BF16) for i in range(2)]
            qib = [cx.sb("qib0", [64, 8, 128], BF16)]
            lo = cx.sb("bs_lo", [128, 1], F32)
            hi = cx.sb("bs_hi", [128, 1], F32)
            mid = cx.sb("bs_mid", [128, 1], F32)
            cnt = cx.sb("bs_cnt", [128, 1], F32)
            ge = cx.sb("bs_ge", [128, 1], F32)
            dd = cx.sb("bs_dd", [128, 2], F32)
            mx = cx.sb("mx", [128, 1], F32)
            mxp = cx.sb("mxp", [128, 8], F32)
            rsp = cx.sb("rsp", [128, 8], F32)
            rsum = cx.sb("rsum", [128, 8], F32)
            Pp = [cx.sb("Pp%d" % i, [128, 512], BF16) for i in range(2)]
            PTp = [cx.sb("PTp%d" % i, [128, 4, 128], BF16) for i in range(2)]
            osb = [cx.sb("osb0", [128, 8, 128], F32)]
            pq = [cx.ps("pq%d" % i, [128, 512], F32) for i in range(3)]
            ptp = [cx.ps("ptp%d" % i, [128, 4, 128], BF16) for i in range(2)]
            po = cx.ps("po", [128, 1024], F32)
            pqc = 0
            ppc = 0
            rlc = 0
            for i in range(nqb):
                S = (2 * i + 2) * 128
                pieces = [(c0, min(512, S - c0)) for c0 in range(0, S, 512)]
                qbt, qibt = qb[i % 2], qib[0]
                cx.dma(SP, qst.ap, qT_d.rearrange("(h d) t -> d h t", d=128)[:, :, i * 128:(i + 1) * 128], writes=[qst])
                cx.op(ACT, lambda: nc.scalar.copy(out=qbt.ap[:], in_=qst.ap[:]), reads=[qst], writes=[qbt])
                cx.dma(SP, qst.ap[0:64], qiT_d.rearrange("(h d) t -> d h t", d=64)[:, :, i * 128:(i + 1) * 128], writes=[qst])
                cx.op(ACT, lambda: nc.scalar.copy(out=qibt.ap[:], in_=qst.ap[0:64]), reads=[qst], writes=[qibt])
                for (c0, n) in pieces:
                    for hd in range(8):
                        pl = pq[pqc % 3]
                        pqc += 1
                        r = rl[rlc % 2]
                        rlc += 1
                        cx.op(PE, lambda: nc.tensor.matmul(pl.ap[:, 0:n], lhsT=qibt.ap[:, hd, :], rhs=kiT.ap[:, c0:c0 + n], start=True, stop=True),
                              reads=[qibt, kiT], writes=[pl])
                        cx.op(ACT, lambda: nc.scalar.activation(out=r.ap[:, 0:n], in_=pl.ap[:, 0:n], func=AF.Relu), reads=[pl], writes=[r])
                        if hd == 0:
                            cx.op(DVE, lambda: nc.vector.tensor_scalar(out=score.ap[:, c0:c0 + n], in0=r.ap[:, 0:n], scalar1=wsc.ap[:, i, 0:1],
                                                                       scalar2=None, op0=ALU.mult), reads=[r, wsc], writes=[score])
                        else:
                            cx.op(DVE, lambda: nc.vector.scalar_tensor_tensor(out=score.ap[:, c0:c0 + n], in0=r.ap[:, 0:n],
                                                                              scalar=wsc.ap[:, i, hd:hd + 1], in1=score.ap[:, c0:c0 + n],
                                                                              op0=ALU.mult, op1=ALU.add), reads=[r, wsc, score], writes=[score])
                if S > 256:
                    cx.op(DVE, lambda: nc.vector.tensor_reduce(out=lo.ap[:], in_=score.ap[:, 0:S], axis=AX.X, op=ALU.min), reads=[score], writes=[lo])
                cx.op(DVE, lambda: nc.vector.tensor_tensor(out=score.ap[:, S - 256:S], in0=score.ap[:, S - 256:S], in1=cmask.ap[:], op=ALU.add),
                      reads=[score, cmask], writes=[score])
                if S > 256:
                    cx.op(DVE, lambda: nc.vector.tensor_reduce(out=hi.ap[:], in_=score.ap[:, 0:S], axis=AX.X, op=ALU.max), reads=[score], writes=[hi])
                    for it in range(NBISECT):
                        cx.op(DVE, lambda: nc.vector.tensor_scalar(out=mid.ap[:], in0=lo.ap[:], scalar1=hi.ap[:, 0:1], scalar2=0.5,
                                                                   op0=ALU.add, op1=ALU.mult), reads=[lo, hi], writes=[mid])
                        cx.op(DVE, lambda: nc.vector.tensor_scalar(out=mb.ap[:, 0:S], in0=score.ap[:, 0:S], scalar1=mid.ap[:, 0:1], scalar2=0.0,
                                                                   op0=ALU.is_ge, op1=ALU.add, accum_out=cnt.ap[:]),
                              reads=[score, mid], writes=[mb, cnt])
                        cx.op(DVE, lambda: nc.vector.tensor_scalar(out=ge.ap[:], in0=cnt.ap[:], scalar1=255.5, scalar2=None, op0=ALU.is_ge),
                              reads=[cnt], writes=[ge])
                        cx.op(DVE, lambda: nc.vector.tensor_tensor(out=dd.ap[:, 0:1], in0=mid.ap[:], in1=lo.ap[:], op=ALU.subtract),
                              reads=[mid, lo], writes=[dd])
                        cx.op(DVE, lambda: nc.vector.tensor_tensor(out=dd.ap[:, 1:2], in0=hi.ap[:], in1=mid.ap[:], op=ALU.subtract),
                              reads=[mid, hi], writes=[dd])
                        cx.op(DVE, lambda: nc.vector.scalar_tensor_tensor(out=lo.ap[:], in0=dd.ap[:, 0:1], scalar=ge.ap[:, 0:1], in1=lo.ap[:],
                                                                          op0=ALU.mult, op1=ALU.add), reads=[dd, ge, lo], writes=[lo])
                        cx.op(DVE, lambda: nc.vector.scalar_tensor_tensor(out=hi.ap[:], in0=dd.ap[:, 1:2], scalar=ge.ap[:, 0:1], in1=mid.ap[:],
                                                                          op0=ALU.mult, op1=ALU.add), reads=[dd, ge, mid], writes=[hi])
                    th = lo
                else:
                    th = thr0
                cx.op(DVE, lambda: nc.vector.tensor_scalar(out=mb.ap[:, 0:S], in0=score.ap[:, 0:S], scalar1=th.ap[:, 0:1], scalar2=NEG,
                                                           op0=ALU.is_lt, op1=ALU.mult), reads=[score, th], writes=[mb])
                ob = osb[0]
                for hd in range(8):
                    for pidx, (c0, n) in enumerate(pieces):
                        pl = pq[pqc % 3]
                        pqc += 1
                        cx.op(PE, lambda: nc.tensor.matmul(pl.ap[:, 0:n], lhsT=qbt.ap[:, hd, :], rhs=KT.ap[:, hd, c0:c0 + n], start=True, stop=True),
                              reads=[qbt, KT], writes=[pl])
                        cx.op(DVE, lambda: nc.vector.scalar_tensor_tensor(out=work.ap[:, c0:c0 + n], in0=pl.ap[:, 0:n], scalar=ATT_SCALE,
                                                                          in1=mb.ap[:, c0:c0 + n], op0=ALU.mult, op1=ALU.add),
                              reads=[pl, mb], writes=[work])
                    cx.op(DVE, lambda: nc.vector.tensor_reduce(out=mx.ap[:], in_=work.ap[:, 0:S], axis=AX.X, op=ALU.max), reads=[work], writes=[mx])
                    cx.op(DVE, lambda: nc.vector.tensor_scalar(out=mx.ap[:], in0=mx.ap[:], scalar1=-1.0, scalar2=None, op0=ALU.mult),
                          reads=[mx], writes=[mx])
                    ntile_tot = S // 128
                    tdone = 0
                    for pi, (c0, n) in enumerate(pieces):
                        P_ = Pp[ppc % 2]
                        PT_ = PTp[ppc % 2]
                        pt_ = ptp[ppc % 2]
                        ppc += 1
                        cx.op(ACT, lambda: nc.scalar.activation(out=P_.ap[:, 0:n], in_=work.ap[:, c0:c0 + n], func=AF.Exp, bias=mx.ap[:, 0:1],
                                                                accum_out=rsp.ap[:, pi:pi + 1]), reads=[work, mx], writes=[P_, rsp])
                        nj = n // 128
                        for j in range(nj):
                            cx.op(PE, lambda: nc.tensor.transpose(out=pt_.ap[:, j, :], in_=P_.ap[:, j * 128:(j + 1) * 128], identity=identb.ap[:]),
                                  reads=[P_, identb], writes=[pt_])
                        if ppc % 2 == 0:
                            cx.op(ACT, lambda: nc.scalar.copy(out=PT_.ap[:, 0:nj, :], in_=pt_.ap[:, 0:nj, :]), reads=[pt_], writes=[PT_])
                        else:
                            cx.op(DVE, lambda: nc.vector.tensor_copy(out=PT_.ap[:, 0:nj, :], in_=pt_.ap[:, 0:nj, :]), reads=[pt_], writes=[PT_])
                        for j in range(nj):
                            tile_idx = c0 // 128 + j
                            cx.op(PE, lambda: nc.tensor.matmul(po.ap[:, hd * 128:(hd + 1) * 128], lhsT=PT_.ap[:, j, :],
                                                               rhs=V.ap[:, tile_idx, hd * 128:(hd + 1) * 128],
                                                               start=(tdone == 0), stop=(tdone == ntile_tot - 1)),
                                  reads=[PT_, V], writes=[po])
                            tdone += 1
                    cx.op(DVE, lambda: nc.vector.tensor_reduce(out=rsum.ap[:, hd:hd + 1], in_=rsp.ap[:, 0:len(pieces)], axis=AX.X, op=ALU.add),
                          reads=[rsp], writes=[rsum])
                cx.op(DVE, lambda: nc.vector.reciprocal(out=rsum.ap[:], in_=rsum.ap[:]), reads=[rsum], writes=[rsum])
                for half in range(2):
                    cx.op(DVE, lambda: nc.vector.tensor_tensor(out=ob.ap[:, half * 4:(half + 1) * 4, :],
                                                               in0=po.ap[:, half * 512:(half + 1) * 512].rearrange("p (h d) -> p h d", h=4),
                                                               in1=rsum.ap[:, half * 4:(half + 1) * 4, None].to_broadcast([128, 4, 128]), op=ALU.mult),
                          reads=[po, rsum], writes=[ob])
                cx.dma(SP, o_d[i * 128:(i + 1) * 128, :], ob.ap[:].rearrange("p h d -> p (h d)"), reads=[ob])
            cx.finish(osb)


_PROGS = {}


def _prog(key, fn):
    if key not in _PROGS:
        _PROGS[key] = fn()
    return _PROGS[key]


def _run(prog, in_maps):
    res = run_bass_kernel_spmd(prog.nc, in_maps, core_ids=list(range(NCORES)))
    return res.results


def _c(a):
    return np.ascontiguousarray(a)


def kernel(x, positions, norm_ffn1, w_ffn1_in, w_ffn1_out, norm_mix, w_mix_in, conv_w, conv_b, dt_bias, a_log,
           d_skip, ssd_norm, w_mix_out, norm_ffn2, w_ffn2_in, w_ffn2_out, final_norm):
    f32 = np.float32
    x = np.asarray(x, f32)
    positions = np.asarray(positions, np.int32)
    HT = SEQ // 2
    cores = [(c // 2, c % 2) for c in range(NCORES)]
    h = [_c(x[b, hf * HT:(hf + 1) * HT]) for (b, hf) in cores]
    pos = [_c(positions[b, hf * HT:(hf + 1) * HT]) for (b, hf) in cores]
    tri = np.where(np.arange(128)[None, :] <= np.arange(128)[:, None], 0.0, NEG).astype(f32)
    cmasks = [np.concatenate([tri, np.full((128, 128), NEG, f32)], 1), np.concatenate([np.zeros((128, 128), f32), tri], 1)]
    yT = None
    out = None
    for l in range(DEPTH + 1):
        stages = []
        ins = [dict(h_in=h[c]) for c in range(NCORES)]
        nf = 0

        def add_ffn(g, wi, wo):
            nonlocal nf
            for d in ins:
                d["ffn_g%d" % nf] = _c(np.asarray(g, f32))
                d["ffn_wi%d" % nf] = wi
                d["ffn_wo%d" % nf] = _c(np.asarray(wo, f32))
            nf += 1
            stages.append(("ffn",))
        if l > 0:
            stages.append(("mixout",))
            for c, d in enumerate(ins):
                d["yT"] = yT[c]
                d["wmo"] = _c(np.asarray(w_mix_out[l - 1], f32))
            add_ffn(norm_ffn2[l - 1], layout_ffn_wi(w_ffn2_in[l - 1]), w_ffn2_out[l - 1])
        if l < DEPTH:
            add_ffn(norm_ffn1[l], layout_ffn_wi(w_ffn1_in[l]), w_ffn1_out[l])
            stages.append(("proj",))
            wmi_l = layout_wmi(w_mix_in[l])
            for c, d in enumerate(ins):
                d["gm"] = _c(np.asarray(norm_mix[l], f32))
                d["wmi"] = wmi_l
                d["pos"] = pos[c]
        else:
            stages.append(("final",))
            for d in ins:
                d["gf"] = _c(np.asarray(final_norm, f32))
        key = ("tok",) + tuple(s[0] for s in stages)
        prog = _prog(key, lambda: TokProg(HT, stages))
        res = _run(prog, ins)
        h = [res[c]["h_out"] for c in range(NCORES)]
        if l == DEPTH:
            out = np.stack([np.concatenate([h[2 * b], h[2 * b + 1]], 0) for b in range(BATCH)], 0)
            break
        u = [np.concatenate([res[2 * b]["u"], res[2 * b + 1]["u"]], 0) for b in range(BATCH)]
        cwl, cbl = np.asarray(conv_w[l], f32), np.asarray(conv_b[l], f32)
        sins = []
        for (b, g) in cores:
            ch = np.concatenate([np.arange(g * 512, (g + 1) * 512), 1024 + g * 128 + np.arange(128), 1280 + g * 128 + np.arange(128)])
            ub = u[b]
            sins.append(dict(xbcT=_c(ub[:, O_XBC + ch].T), cw=_c(cwl[:, ch].T), cb=_c(cbl[ch]),
                             z=_c(ub[:, O_Z + g * 512:O_Z + (g + 1) * 512]), dtr=_c(ub[:, O_DT + g * 8:O_DT + (g + 1) * 8]),
                             dtb=_c(np.asarray(dt_bias[l], f32)[g * 8:(g + 1) * 8]), alog=_c(np.asarray(a_log[l], f32)[g * 8:(g + 1) * 8]),
                             dsk=_c(np.asarray(d_skip[l], f32)[g * 8:(g + 1) * 8]), ng=_c(np.asarray(ssd_norm[l], f32)[g * 512:(g + 1) * 512])))
        sres = _run(_prog(("ssd",), lambda: SsdProg(SEQ)), sins)
        dins = []
        toks = []
        for (b, par) in cores:
            tok = np.concatenate([np.arange((2 * i + par) * 128, (2 * i + par + 1) * 128) for i in range(SEQ // 256)])
            toks.append(tok)
            ub = u[b]
            dins.append(dict(qT=_c(ub[tok, O_Q:O_K].T), kT=_c(ub[:, O_K:O_V].T), v=_c(ub[:, O_V:O_QI]),
                             qiT=_c(ub[tok, O_QI:O_KI].T), kiT=_c(ub[:, O_KI:O_WI].T), wi=_c(ub[tok, O_WI:O_WI + 8]),
                             cmask=cmasks[par]))
        dres = _run(_prog(("dsa",), lambda: DsaProg(SEQ)), dins)
        yT = []
        ys = []
        for b in range(BATCH):
            y = np.empty((SEQ, 2048), f32)
            for g in range(2):
                y[:, g * 512:(g + 1) * 512] = sres[2 * b + g]["y"]
                y[toks[2 * b + g], 1024:] = dres[2 * b + g]["o"]
            ys.append(y)
        for (b, hf) in cores:
            yT.append(_c(ys[b][hf * HT:(hf + 1) * HT].T))
    return out.astype(np.float32)
```
